# Optimizing a Trainium2 kernel written in Bass

```python
import jax, jax.numpy as jnp
from jax import lax
import numpy as np

D_MODEL = 1024
BATCH = 4
SEQ = 8192
DEPTH = 4

MEM_LEN = 256
CONV_WIDTH = 256
CONV_K = 3
RET_HEADS = 4
RET_QK_DIM = 64
RET_V_DIM = 128
RET_CHUNK = 128
RET_QK = RET_HEADS * RET_QK_DIM
RET_V = RET_HEADS * RET_V_DIM
ROPE_BASE = 10000.0
SG_GROUPS = 4
SG_GROUP_DIM = 64
SG_WIDTH = SG_GROUPS * SG_GROUP_DIM
SG_CHUNK = 128
N_BRANCH = 3
XA_HEADS = 4
XA_HEAD_DIM = D_MODEL // XA_HEADS
D_FF = -(-8 * D_MODEL // (3 * 256)) * 256
EPS = 1e-6

IN_WIDTHS = (CONV_WIDTH, CONV_WIDTH, CONV_WIDTH,
             RET_QK, RET_QK, RET_V, RET_V,
             2 * SG_WIDTH,
             N_BRANCH * D_MODEL)
IN_COLS = int(sum(IN_WIDTHS))
IN_SPLITS = [int(s) for s in np.cumsum(IN_WIDTHS)[:-1]]

kernel_name = "hybrid_conv_retention_sgu_xattn_block"


def rmsnorm(x, g):
    xf = x.astype(jnp.float32)
    y = xf * lax.rsqrt(jnp.mean(xf * xf, axis=-1, keepdims=True) + EPS)
    return (y * g.astype(jnp.float32)).astype(x.dtype)


def layernorm(x, g):
    xf = x.astype(jnp.float32)
    mu = jnp.mean(xf, axis=-1, keepdims=True)
    var = jnp.mean(jnp.square(xf - mu), axis=-1, keepdims=True)
    return ((xf - mu) * lax.rsqrt(var + EPS) * g.astype(jnp.float32)).astype(x.dtype)


def short_conv_mixer(a_x, a_c, a_b, conv_w):
    z = a_c * a_x
    S = z.shape[1]
    zp = jnp.pad(z, ((0, 0), (CONV_K - 1, 0), (0, 0)))
    y = sum(conv_w[k] * zp[:, k:k + S] for k in range(CONV_K))
    return a_b * y


def rotary(x):
    S, Dh = x.shape[1], x.shape[-1]
    half = Dh // 2
    inv = ROPE_BASE ** (-jnp.arange(half, dtype=jnp.float32) / half)
    ang = jnp.arange(S, dtype=jnp.float32)[:, None] * inv[None, :]
    cos = jnp.cos(ang)[None, :, None, :]
    sin = jnp.sin(ang)[None, :, None, :]
    xf = x.astype(jnp.float32)
    x1, x2 = xf[..., :half], xf[..., half:]
    return jnp.concatenate([x1 * cos - x2 * sin, x1 * sin + x2 * cos], axis=-1)


def retention(q, k, v, gn_gain):
    B, S, H, dk = q.shape
    dv = v.shape[-1]
    C = RET_CHUNK
    N = S // C
    out_dtype = v.dtype
    gamma = 1.0 - jnp.exp2(-5.0 - jnp.arange(H, dtype=jnp.float32))
    log_g = jnp.log(gamma)
    q = q.reshape(B, N, C, H, dk)
    k = k.reshape(B, N, C, H, dk) * (dk ** -0.5)
    v = v.astype(jnp.float32).reshape(B, N, C, H, dv)
    idx = jnp.arange(C, dtype=jnp.float32)
    diff = idx[:, None] - idx[None, :]
    decay = jnp.where(diff >= 0, jnp.exp(log_g[:, None, None] * jnp.maximum(diff, 0.0)), 0.0)
    scores = jnp.einsum('bnchd,bnmhd->bnhcm', q, k) * decay[None, None]
    inner = jnp.einsum('bnhcm,bnmhe->bnche', scores, v)
    zeta = jnp.exp(log_g[:, None] * (C - 1 - idx)[None, :])
    kv = jnp.einsum('bnmhd,bnmhe->nbhde', k, v * zeta.T[None, None, :, :, None])
    chunk_decay = jnp.exp(log_g * C)[None, :, None, None]

    def step(R, kv_n):
        return chunk_decay * R + kv_n, R

    _, R_prev = lax.scan(step, jnp.zeros((B, H, dk, dv), jnp.float32), kv)
    xi = jnp.exp(log_g[:, None] * (idx + 1.0)[None, :])
    cross = jnp.einsum('bnchd,nbhde->bnche', q, R_prev) * xi.T[None, None, :, :, None]
    o = (inner + cross).reshape(B, S, H, dv)
    mu = jnp.mean(o, axis=-1, keepdims=True)
    var = jnp.mean(jnp.square(o - mu), axis=-1, keepdims=True)
    o = ((o - mu) * lax.rsqrt(var + EPS)).reshape(B, S, H * dv) * gn_gain.astype(jnp.float32)
    return o.astype(out_dtype)


def spatial_gating(z, ln_g, w_s, b_s):
    B, S, _ = z.shape
    C = SG_CHUNK
    N = S // C
    u, v = jnp.split(z, 2, axis=-1)
    v = layernorm(v, ln_g).reshape(B, N, C, SG_GROUPS, SG_GROUP_DIM)
    mask = jnp.tril(jnp.ones((C, C), dtype=w_s.dtype))
    vm = jnp.einsum('gcm,bnmgd->bncgd', w_s * mask[None], v) + b_s.T[None, None, :, :, None]
    return u * vm.reshape(B, S, SG_WIDTH)


def cross_attention(h, mm, wq, wk, wv, wo):
    B, S, _ = h.shape
    M = mm.shape[1]
    q = (h @ wq).reshape(B, S, XA_HEADS, XA_HEAD_DIM)
    k = (mm @ wk).reshape(B, M, XA_HEADS, XA_HEAD_DIM)
    v = (mm @ wv).reshape(B, M, XA_HEADS, XA_HEAD_DIM)
    s = jnp.einsum('bshd,bmhd->bhsm', q, k).astype(jnp.float32) * (XA_HEAD_DIM ** -0.5)
    p = jax.nn.softmax(s, axis=-1).astype(v.dtype)
    o = jnp.einsum('bhsm,bmhd->bshd', p, v).reshape(B, S, D_MODEL)
    return o @ wo


def setup_inputs(seed: int = 0) -> dict:
    key = jax.random.key(seed)
    ks = jax.random.split(key, 32)
    L = DEPTH
    f32 = jnp.float32

    def w(k, shape, fan_in):
        return jax.random.normal(k, shape, f32) * (fan_in ** -0.5)

    def gain(k, shape):
        return 1.0 + 0.02 * jax.random.normal(k, shape, f32)

    return {
        "x": jax.random.normal(ks[0], (BATCH, SEQ, D_MODEL), f32),
        "mem": jax.random.normal(ks[1], (BATCH, MEM_LEN, D_MODEL), f32),
        "norm_mix": gain(ks[2], (L, D_MODEL)),
        "w_in": w(ks[3], (L, D_MODEL, IN_COLS), D_MODEL),
        "conv_w": 0.5 * jax.random.normal(ks[4], (L, CONV_K, CONV_WIDTH), f32),
        "w_a_out": w(ks[5], (L, CONV_WIDTH, D_MODEL), CONV_WIDTH),
        "ret_gn": gain(ks[6], (L, RET_V)),
        "w_b_out": w(ks[7], (L, RET_V, D_MODEL), RET_V),
        "sg_ln": gain(ks[8], (L, SG_WIDTH)),
        "sg_w": w(ks[9], (L, SG_GROUPS, SG_CHUNK, SG_CHUNK), SG_CHUNK),
        "sg_b": 1.0 + 0.1 * jax.random.normal(ks[10], (L, SG_GROUPS, SG_CHUNK), f32),
        "w_c_out": w(ks[11], (L, SG_WIDTH, D_MODEL), SG_WIDTH),
        "w_o": w(ks[12], (L, D_MODEL, D_MODEL), D_MODEL),
        "norm_xa": gain(ks[13], (L, D_MODEL)),
        "norm_mem": gain(ks[14], (L, D_MODEL)),
        "xa_q": w(ks[15], (L, D_MODEL, D_MODEL), D_MODEL),
        "xa_k": w(ks[16], (L, D_MODEL, D_MODEL), D_MODEL),
        "xa_v": w(ks[17], (L, D_MODEL, D_MODEL), D_MODEL),
        "xa_o": w(ks[18], (L, D_MODEL, D_MODEL), D_MODEL),
        "norm_ffn": gain(ks[19], (L, D_MODEL)),
        "w1": w(ks[20], (L, D_MODEL, D_FF), D_MODEL),
        "w3": w(ks[21], (L, D_MODEL, D_FF), D_MODEL),
        "w2": w(ks[22], (L, D_FF, D_MODEL), D_FF),
        "final_norm": gain(ks[23], (D_MODEL,)),
    }


def reference(x, mem, norm_mix, w_in, conv_w, w_a_out, ret_gn, w_b_out, sg_ln, sg_w, sg_b,
              w_c_out, w_o, norm_xa, norm_mem, xa_q, xa_k, xa_v, xa_o, norm_ffn, w1, w3, w2,
              final_norm):
    B, S, _ = x.shape
    for l in range(DEPTH):
        h = rmsnorm(x, norm_mix[l])
        p = h @ w_in[l]
        a_x, a_c, a_b, q, k, v, g, z, gates = jnp.split(p, IN_SPLITS, axis=-1)
        y_a = short_conv_mixer(a_x, a_c, a_b, conv_w[l]) @ w_a_out[l]
        qr = rotary(q.reshape(B, S, RET_HEADS, RET_QK_DIM))
        kr = rotary(k.reshape(B, S, RET_HEADS, RET_QK_DIM))
        r = retention(qr, kr, v.reshape(B, S, RET_HEADS, RET_V_DIM), ret_gn[l])
        y_b = (jax.nn.silu(g) * r) @ w_b_out[l]
        y_c = spatial_gating(jax.nn.gelu(z), sg_ln[l], sg_w[l], sg_b[l]) @ w_c_out[l]
        g_a, g_b, g_c = jnp.split(jax.nn.sigmoid(gates), N_BRANCH, axis=-1)
        merged = g_a * y_a + g_b * y_b + g_c * y_c
        x = x + merged @ w_o[l]
        h = rmsnorm(x, norm_xa[l])
        mm = rmsnorm(mem, norm_mem[l])
        x = x + cross_attention(h, mm, xa_q[l], xa_k[l], xa_v[l], xa_o[l])
        h = rmsnorm(x, norm_ffn[l])
        x = x + (jax.nn.silu(h @ w1[l]) * (h @ w3[l])) @ w2[l]
    return rmsnorm(x, final_norm)
```

```python
import math
import numpy as np
import concourse.bass as bass
import concourse.mybir as mybir
from concourse.bass_utils import run_bass_kernel_spmd

F32 = mybir.dt.float32
BF16 = mybir.dt.bfloat16
ALU = mybir.AluOpType
AF = mybir.ActivationFunctionType

D = 1024
KC = 8
DEPTH = 4
NCORES = 8
GT = 512
MT = 256
NCH = MT // 128
NSUB = GT // MT
DFF = 2816
FC = DFF // 128
EPS = 1e-6
import os as _os
DBG = set(_os.environ.get("KDBG", "").split(","))
SLOT_E = 2048
NSLOT = 4
MEM_LEN = 256

BLK = {}
_names = []


def _add(n):
    BLK[n] = len(_names)
    _names.append(n)


for _n in ["qk", "k", "v0", "v1", "g0", "g1", "ax", "ac", "ab", "zu", "zv"]:
    _add(_n)
for _o in range(8):
    _add("gab%d" % _o)
    _add("gco%d" % _o)
for _j in range(4):
    _add("wo%d" % _j)
for _j in range(4):
    _add("xq%d" % _j)
for _j in range(4):
    _add("xo%d" % _j)
for _j in range(11):
    _add("w1_%d" % _j)
    _add("w3_%d" % _j)
for _o in range(8):
    _add("w2a%d" % _o)
    _add("w2b%d" % _o)
for _j in range(4):
    _add("xk%d" % _j)
for _j in range(4):
    _add("xv%d" % _j)
_add("wm")
NBLK = len(_names)
P1_BLOCKS = ["k", "v0", "v1", "ax", "ac"]

C_AX, C_AC, C_AB = 0, 256, 512
C_Q, C_K, C_V, C_G = 768, 1024, 1280, 1792
C_Z = 2304
C_GATE = 2816


def _kmaj(w):
    K, n = w.shape
    return np.ascontiguousarray(w.reshape(K // 128, 128, n).transpose(1, 0, 2)).reshape(128, -1)


def pack_layer(w_in, w_a_out, w_b_out, w_c_out, w_o, xa_q, xa_k, xa_v, xa_o, w1, w3, w2, sg_w):
    pk = np.zeros((NBLK, 128, SLOT_E), np.float32)

    def put(name, arr):
        pk[BLK[name], :, :arr.shape[1]] = arr

    hA = np.concatenate([np.arange(h * 64, h * 64 + 32) for h in range(4)])
    hB = hA + 32
    put("qk", _kmaj(w_in[:, np.concatenate([C_Q + hA, C_Q + hB])]))
    put("k", _kmaj(w_in[:, np.concatenate([C_K + hA, C_K + hB])]))
    put("v0", _kmaj(w_in[:, C_V:C_V + 256]))
    put("v1", _kmaj(w_in[:, C_V + 256:C_V + 512]))
    put("g0", _kmaj(w_in[:, C_G:C_G + 256]))
    put("g1", _kmaj(w_in[:, C_G + 256:C_G + 512]))
    put("ax", _kmaj(w_in[:, C_AX:C_AX + 256]))
    put("ac", _kmaj(w_in[:, C_AC:C_AC + 256]))
    put("ab", _kmaj(w_in[:, C_AB:C_AB + 256]))
    put("zu", _kmaj(w_in[:, C_Z:C_Z + 256]))
    put("zv", _kmaj(w_in[:, C_Z + 256:C_Z + 512]))
    for o in range(8):
        ga = w_in[:, C_GATE + o * 128:C_GATE + (o + 1) * 128]
        gb = w_in[:, C_GATE + 1024 + o * 128:C_GATE + 1024 + (o + 1) * 128]
        gc = w_in[:, C_GATE + 2048 + o * 128:C_GATE + 2048 + (o + 1) * 128]
        put("gab%d" % o, _kmaj(np.concatenate([ga, gb], axis=1)))
        outs = np.concatenate([w_a_out[:, o * 128:(o + 1) * 128],
                               w_c_out[:, o * 128:(o + 1) * 128],
                               w_b_out[:, o * 128:(o + 1) * 128]], axis=0)
        blk = np.concatenate([_kmaj(gc), _kmaj(outs)], axis=1)
        put("gco%d" % o, blk)
    for j in range(4):
        put("wo%d" % j, _kmaj(w_o[:, j * 256:(j + 1) * 256]))
        put("xq%d" % j, _kmaj(xa_q[:, j * 256:(j + 1) * 256]))
        put("xo%d" % j, _kmaj(xa_o[:, j * 256:(j + 1) * 256]))
        put("xk%d" % j, _kmaj(xa_k[:, j * 256:(j + 1) * 256]))
        put("xv%d" % j, _kmaj(xa_v[:, j * 256:(j + 1) * 256]))
    for j in range(11):
        put("w1_%d" % j, _kmaj(w1[:, j * 256:(j + 1) * 256]))
        put("w3_%d" % j, _kmaj(w3[:, j * 256:(j + 1) * 256]))
    for o in range(8):
        put("w2a%d" % o, _kmaj(w2[0:1536, o * 128:(o + 1) * 128]))
        put("w2b%d" % o, _kmaj(w2[1536:2816, o * 128:(o + 1) * 128]))
    put("wm", np.ascontiguousarray(sg_w.transpose(2, 0, 1)).reshape(128, 512))
    return pk


SP_GMIX, SP_GXA, SP_GFFN, SP_GMEM = 0, 8, 16, 24
SP_CONV = 32
SP_GN = 38
SP_LN = 42
SP_SGB = 298
NSP = 810


def pack_small(norm_mix, norm_xa, norm_ffn, norm_mem, conv_w, ret_gn, sg_ln, sg_b):
    sp = np.zeros((128, NSP), np.float32)
    for off, g in ((SP_GMIX, norm_mix), (SP_GXA, norm_xa), (SP_GFFN, norm_ffn), (SP_GMEM, norm_mem)):
        sp[:, off:off + 8] = g.reshape(8, 128).T
    sp[:, SP_CONV:SP_CONV + 6] = conv_w.reshape(3, 2, 128).transpose(2, 1, 0).reshape(128, 6)
    sp[:, SP_GN:SP_GN + 4] = ret_gn.reshape(4, 128).T
    sp[:, SP_LN:SP_LN + 256] = sg_ln[None, :]
    sp[:, SP_SGB:SP_SGB + 512] = sg_b.reshape(1, 512)
    return sp


CS_G = 0
CS_MASK = 8
CS_ID = 520
CS_FN = 648
CS_SEL = 656
NCST = 664


def make_consts(core, final_norm):
    cs = np.zeros((128, NCST), np.float32)
    gam = 1.0 - np.exp2(-5.0 - np.arange(4, dtype=np.float64))
    cs[:, CS_G] = np.repeat(gam ** 128, 32)
    m = np.arange(128)[:, None]
    c = np.arange(128)[None, :]
    cs[:, CS_MASK:CS_MASK + 512] = np.tile((c >= m).astype(np.float32), (1, 4))
    cs[:, CS_ID:CS_ID + 128] = np.eye(128, dtype=np.float32)
    cs[:, CS_FN:CS_FN + 8] = final_norm.reshape(8, 128).T
    if core % 2 == 1:
        cs[:, CS_SEL + core - 1] = 1.0
    return cs


def make_tables(pos0, nt):
    half = 32
    inv = (np.float32(10000.0) ** (-(np.arange(half, dtype=np.float32)) / np.float32(half))).astype(np.float32)
    pos = np.arange(pos0, pos0 + nt, dtype=np.float32)
    ang = (pos[None, :] * inv[:, None]).astype(np.float32).astype(np.float64)
    cos = np.cos(ang)
    sin = np.sin(ang)
    gam = 1.0 - np.exp2(-5.0 - np.arange(4, dtype=np.float64))
    cidx = (np.arange(nt) % 128).astype(np.float64)
    tab = np.zeros((4, 128, nt), np.float64)
    for h in range(4):
        dq = gam[h] ** (cidx + 1.0)
        dk = gam[h] ** (-(cidx + 1.0)) * (64.0 ** -0.5)
        sl = slice(h * 32, (h + 1) * 32)
        tab[0, sl] = cos * dq[None, :]
        tab[1, sl] = sin * dq[None, :]
        tab[2, sl] = cos * dk[None, :]
        tab[3, sl] = sin * dk[None, :]
    return tab.astype(np.float32)


class Tok:
    __slots__ = ("sem", "val", "eng")

    def __init__(self, sem, val, eng):
        self.sem, self.val, self.eng = sem, val, eng


SEM_EPOCH = 2500


class Eng:
    def __init__(self, name, sem, nc=None):
        self.name = name
        self.sem = sem
        self.nc = nc
        self.nep = 0
        self.count = 0
        self.prog = []
        self.waited = {}
        self.pending_reads = []


class TV:
    def __init__(self, ap, space, lo, d1, d2, esz):
        self.ap, self.space, self.lo, self.d1, self.d2, self.esz = ap, space, lo, d1, d2, esz

    def reg(self, a=0, b=None):
        b = self.d1 if b is None else b
        return (self.space, self.lo + a * self.d2 * self.esz, self.lo + b * self.d2 * self.esz)


class Ctx:
    NDSEM = 16

    def __init__(self, nc):
        self.nc = nc
        self.E = {}
        for n in ("pe", "act", "dve", "pool", "sp"):
            self.E[n] = Eng(n, nc.alloc_semaphore("s_" + n), nc)
        self.dsem = [nc.alloc_semaphore("d%d" % i) for i in range(self.NDSEM)]
        self.dcount = 0
        self.recs = {}
        self.ps_i = 0

    def _overl(self, reg):
        sp, lo, hi = reg
        return [r for r in self.recs.get(sp, ()) if r[0] < hi and lo < r[1]]

    def _rec(self, reg):
        sp, lo, hi = reg
        lst = self.recs.setdefault(sp, [])
        for r in lst:
            if r[0] == lo and r[1] == hi:
                return r
        r = [lo, hi, None, []]
        lst.append(r)
        return r

    def _wait(self, E, tok):
        if tok is None:
            return
        if tok.eng == "pe" and E.name == "pe":
            return
        key = id(tok.sem)
        if E.waited.get(key, 0) >= tok.val:
            return
        E.waited[key] = tok.val
        sem, val = tok.sem, tok.val
        E.prog.append(lambda e, sem=sem, val=val: e.wait_ge(sem, val))

    def _deps(self, E, reads, writes):
        for reg in reads:
            for r in self._overl(reg):
                self._wait(E, r[2])
        for reg in writes:
            for r in self._overl(reg):
                self._wait(E, r[2])
                for t in r[3]:
                    self._wait(E, t)

    def _commit(self, tok, reads, writes):
        for reg in reads:
            self._rec(reg)[3].append(tok)
        for reg in writes:
            me = self._rec(reg)
            for r in self._overl(reg):
                r[2] = tok
                r[3] = []
            me[2] = tok
            me[3] = []

    def op(self, eng, fn, reads=(), writes=(), inc=True):
        E = self.E[eng]
        if eng == "pe":
            self.pe_n = getattr(self, "pe_n", 0) + 1
        self._deps(E, reads, writes)
        if inc:
            self.bump(E)
            tok = Tok(E.sem, E.count, eng)
            sem = E.sem
            E.prog.append(lambda e, fn=fn, sem=sem: fn(e).then_inc(sem, 1))
            rd = list(reads) + E.pending_reads
            E.pending_reads = []
            self._commit(tok, rd, writes)
        else:
            assert not writes or eng == "pe"
            E.prog.append(lambda e, fn=fn: fn(e))
            E.pending_reads += list(reads)

    def bump(self, E):
        if E.count >= SEM_EPOCH:
            E.nep += 1
            E.sem = self.nc.alloc_semaphore("s_%s_%d" % (E.name, E.nep))
            E.count = 0
        E.count += 1

    def dma(self, eng, out, in_, reads=(), writes=()):
        E = self.E[eng]
        i = self.dcount
        self.dcount += 1
        sem = self.dsem[i % self.NDSEM]
        k = i // self.NDSEM
        if k > 0:
            self._wait(E, Tok(sem, 16 * k, "dma"))
        self._deps(E, reads, writes)
        tok = Tok(sem, 16 * (k + 1), "dma")
        E.prog.append(lambda e, out=out, in_=in_, sem=sem: e.dma_start(out=out, in_=in_).then_inc(sem, 16))
        self._commit(tok, reads, writes)
        return tok

    def wait_all(self, eng, regs):
        E = self.E[eng]
        self._deps(E, regs, regs)


class Builder:
    def __init__(self, nt, mode, nlayers=1, seq=None, ncores=NCORES):
        self.dry = seq is None
        self.ncores = ncores
        self.ws_rec = []
        self.ws_seq = seq
        self.ws_issued = 0
        self.ws_used = 0
        self.nt = nt
        self.ng = nt // GT
        self.mode = mode
        if mode == "p1":
            self.bmap = {BLK[n]: i for i, n in enumerate(P1_BLOCKS)}
        else:
            self.bmap = {i: i for i in range(NBLK)}
        self.nb = len(self.bmap)
        self.nlayers = nlayers
        nc = self.nc = bass.Bass("TRN2", target_bir_lowering=False)
        self.c = Ctx(nc)
        self.sb_off = 0
        self.sb_cap = 206 * 1024
        self.sb = nc.alloc_sbuf_tensor("sb", [128, self.sb_cap // 2], BF16)
        self.psum = []
        for i in range(8):
            t = nc.alloc_psum_tensor("ps%d" % i, [128, 512], F32)
            self.psum.append(TV(t[:, :].rearrange("p (a b) -> p a b", a=1), "ps%d" % i, 0, 1, 512, 4))
        self.build()

    def alloc(self, d1, d2, dt, at=None):
        esz = 4 if dt == F32 else 2
        nb = d1 * d2 * esz
        if at is None:
            lo = (self.sb_off + 63) // 64 * 64
            self.sb_off = lo + nb
        else:
            lo = at
        assert lo + nb <= self.sb_cap, ("SBUF overflow", lo, nb)
        ap = self.sb[:, lo // 2:(lo + nb) // 2]
        if dt == F32:
            ap = ap.bitcast(F32)
        ap = ap.rearrange("p (a b) -> p a b", a=d1)
        return TV(ap, "sb", lo, d1, d2, esz)

    def ps_next(self):
        t = self.psum[self.c.ps_i % 8]
        self.c.ps_i += 1
        return t

    @staticmethod
    def ps_bf(ps):
        return ps.ap[:, 0, :].bitcast(BF16)

    def ws_init(self, seq):
        self.ws_seq = seq
        self.ws_issued = 0
        self.ws_used = 0

    def ws_issue_upto(self, n):
        n = min(n, len(self.ws_seq))
        while self.ws_issued < n:
            i = self.ws_issued
            l, b = self.ws_seq[i]
            slot = self.slots[i % NSLOT]
            r = l * self.nb + self.bmap[b]
            src = self.wbf[r]
            self.c.dma("sp", slot.ap[:, 0, :], src, reads=[("wbf", r, r + 1)], writes=[slot.reg()])
            self.ws_issued += 1

    def ws_get(self, l, name):
        if self.dry:
            self.ws_rec.append((l, BLK[name]))
            return self.slots[0]
        i = self.ws_used
        assert self.ws_seq[i] == (l, BLK[name]), (self.ws_seq[i], l, name)
        self.ws_issue_upto(i + NSLOT - 1)
        self.ws_used += 1
        return self.slots[i % NSLOT]

    def mm(self, ps, out_ap, pairs, reads, first_writes=True, last=True, start=True):
        n = len(pairs)
        for i, (l, r) in enumerate(pairs):
            st = start and i == 0
            sp = last and i == n - 1
            fn = (lambda e, l=l, r=r, st=st, sp=sp, out_ap=out_ap:
                  e.matmul(out_ap, lhsT=l, rhs=r, start=st, stop=sp))
            if i == n - 1 and last:
                self.c.op("pe", fn, reads=reads if n == 1 else (), writes=[ps.reg()])
            elif i == 0:
                self.c.op("pe", fn, reads=reads, writes=[ps.reg()] if first_writes else (), inc=False)
            else:
                self.c.op("pe", fn, inc=False)

    def act(self, out, in_, func, reads, writes, **kw):
        self.c.op("act", lambda e: e.activation(out=out, in_=in_, func=func, **kw), reads, writes)

    def tt(self, eng, out, in0, in1, op, reads, writes):
        self.c.op(eng, lambda e: e.tensor_tensor(out=out, in0=in0, in1=in1, op=op), reads, writes)

    def ts(self, eng, out, in0, s1, s2, op0, op1, reads, writes):
        if s2 is None:
            self.c.op(eng, lambda e: e.tensor_scalar(out=out, in0=in0, scalar1=s1, scalar2=None, op0=op0), reads, writes)
        else:
            self.c.op(eng, lambda e: e.tensor_scalar(out=out, in0=in0, scalar1=s1, scalar2=s2, op0=op0, op1=op1), reads, writes)

    def stt(self, eng, out, in0, scalar, in1, op0, op1, reads, writes):
        self.c.op(eng, lambda e: e.scalar_tensor_tensor(out=out, in0=in0, scalar=scalar, in1=in1, op0=op0, op1=op1), reads, writes)

    def cp(self, eng, out, in_, reads, writes):
        if eng == "act":
            self.act(out, in_, AF.Copy, reads, writes)
        else:
            self.c.op(eng, lambda e: e.tensor_copy(out=out, in_=in_), reads, writes)

    def build(self):
        nc, c = self.nc, self.c
        nt, ng, mode = self.nt, self.ng, self.mode
        NL = self.nlayers
        ncores_ = self.ncores
        self.x_in = nc.dram_tensor("x_in", [nt, D], F32, kind="ExternalInput")
        self.cst_d = nc.dram_tensor("cst", [128, NCST], F32, kind="ExternalInput")
        if mode in ("p1", "p2", "fused"):
            NB = self.nb
            self.wpk = nc.dram_tensor("wpk", [NL * NB * 128, SLOT_E], F32, kind="ExternalInput")
            self.spd = nc.dram_tensor("spd", [NL * 128, NSP], F32, kind="ExternalInput")
            self.tabs = nc.dram_tensor("tabs", [4 * 128, nt], F32, kind="ExternalInput")
            wbf_t = nc.dram_tensor("wbf", [NL * NB * 128, SLOT_E], BF16)
            self.wbf_t = wbf_t
            self.wbf = [wbf_t[i * 128:(i + 1) * 128, :] for i in range(NL * NB)]
        if mode in ("p2", "fused"):
            self.mem_d = nc.dram_tensor("mem", [MEM_LEN, D], F32, kind="ExternalInput")
        if mode == "fused":
            self.cc_in = [nc.dram_tensor("ccin%d" % l, [128, 260], F32) for l in range(NL)]
            self.cc_out = [nc.dram_tensor("ccout%d" % l, [ncores_ * 128, 260], F32) for l in range(NL)]
            self.ccsem = [nc.alloc_semaphore("cc%d" % l) for l in range(NL)]
        if mode == "p2":
            self.st_in = nc.dram_tensor("st_in", [128, 260], F32, kind="ExternalInput")
        if mode == "p1":
            self.st_out = nc.dram_tensor("st_out", [128, 260], F32, kind="ExternalOutput")
        else:
            self.x_out = nc.dram_tensor("x_out", [nt, D], F32, kind="ExternalOutput")

        A = self.alloc
        self.x = A(ng * KC, GT, F32)
        self.cst = A(1, NCST, F32)
        self.maskb = A(1, 512, BF16)
        self.identb = A(1, 128, BF16)
        self.ones_n = A(1, 128, BF16)
        self.ones_g = A(1, 128, BF16)
        self.ones_1 = A(1, 128, BF16)
        if mode != "fin":
            self.spp = A(1, NSP, F32)
            self.sgb_bf = A(1, 512, BF16)
            self.wmT = A(1, 512, BF16)
            self.TA = A(1, 128, F32)
            self.TB = A(1, 128, F32)
            self.RA = A(1, 128, BF16)
            self.RB = A(1, 128, BF16)
            self.QA = A(NCH, 512, BF16)
            self.QB = A(NCH, 512, BF16)
            self.z = A(2, MT + 2, F32)
            self.kmemT = A(8, MEM_LEN, BF16)
            self.vmem = A(2, D, BF16)
            self.slots = [A(1, SLOT_E, BF16) for _ in range(NSLOT)]
            self.stt_sb = A(1, 260, F32)
        self.scr0 = (self.sb_off + 63) // 64 * 64

        c.dma("sp", self.cst.ap[:, 0, :], self.cst_d[:, :], writes=[self.cst.reg()])
        cs = self.cst.ap
        self.cp("pool", self.maskb.ap[:, 0, :], cs[:, 0, CS_MASK:CS_MASK + 512], [self.cst.reg()], [self.maskb.reg()])
        self.cp("pool", self.identb.ap[:, 0, :], cs[:, 0, CS_ID:CS_ID + 128], [self.cst.reg()], [self.identb.reg()])
        for t, v in ((self.ones_n, 1.0 / 1024), (self.ones_g, 1.0 / 128), (self.ones_1, 1.0)):
            c.op("pool", lambda e, t=t, v=v: e.memset(t.ap[:, 0, :], v), (), [t.reg()])
        self.identf = cs[:, 0, CS_ID:CS_ID + 128]

        if mode == "fin":
            self.load_x()
            self.final_norm_store()
        elif mode == "p1":
            self.convert_weights(0, ["k", "v0", "v1", "ax", "ac"])
            self.load_x()
            self.layer_small(0)
            self.zero_state()
            self.p1(0)
            c.dma("sp", self.st_out[:, :], self.stt_sb.ap[:, 0, :], reads=[self.stt_sb.reg()], writes=[("out", 0, 1)])
        elif mode == "p2":
            self.convert_weights(0, None)
            self.load_x()
            self.layer_small(0)
            c.dma("sp", self.stt_sb.ap[:, 0, :], self.st_in[:, :], writes=[self.stt_sb.reg()])
            self.state_from_stt()
            self.p2(0)
            self.store_x()
        elif mode == "fused":
            self.convert_weights(0, first=P1_BLOCKS)
            self.load_x()
            for l in range(NL):
                self.layer_small(l)
                self.zero_state()
                self.mark("p1 L%d" % l)
                self.p1(l)
                if l + 1 < NL:
                    self.convert_weights(l + 1, first=P1_BLOCKS)
                self.exchange(l)
                self.state_from_stt()
                self.p2(l)
            self.final_norm_store()
        c.wait_all("sp", [("out", 0, 1 << 20)])
        if not self.dry:
            assert self.ws_used == len(self.ws_seq)
            self.emit()

    def emit(self):
        nc, c = self.nc, self.c
        with nc.Block() as block:
            @block.tensor
            def _(e):
                for f in c.E["pe"].prog:
                    f(e)

            @block.scalar
            def _(e):
                for f in c.E["act"].prog:
                    f(e)

            @block.vector
            def _(e):
                for f in c.E["dve"].prog:
                    f(e)

            @block.gpsimd
            def _(e):
                for f in c.E["pool"].prog:
                    f(e)

            @block.sync
            def _(e):
                for f in c.E["sp"].prog:
                    f(e)

    def convert_weights(self, l, names=None, first=()):
        c = self.c
        blocks = list(range(self.nb))
        runs = []
        for b in blocks:
            if runs and runs[-1][1] == b and runs[-1][1] - runs[-1][0] < 4:
                runs[-1][1] = b + 1
            else:
                runs.append([b, b + 1])
        if first and self.mode != "p1":
            fb = set(BLK[n] for n in first)
            runs.sort(key=lambda r: 0 if any(b in fb for b in range(r[0], r[1])) else 1)
        for b0, b1 in runs:
            r0 = (l * self.nb + b0) * 128
            r1 = (l * self.nb + b1) * 128
            c.dma("pool", self.wbf_t[r0:r1, :], self.wpk[r0:r1, :], writes=[("wbf", l * self.nb + b0, l * self.nb + b1)])

    def load_x(self):
        c = self.c
        st = [self.alloc(1, D, F32, at=self.scr0 + i * 4096) for i in range(2)]
        for t in range(self.nt // 128):
            s = st[t % 2]
            c.dma("sp", s.ap[:, 0, :], self.x_in[t * 128:(t + 1) * 128, :], writes=[s.reg()])
            g, j = divmod(t, 4)
            for half in range(2):
                ps = self.ps_next()
                for q in range(4):
                    kc = half * 4 + q
                    fn = (lambda e, ps=ps, q=q, s=s, kc=kc:
                          e.transpose(ps.ap[:, 0, q * 128:(q + 1) * 128], s.ap[:, 0, kc * 128:(kc + 1) * 128], self.identf))
                    self.c.op("pe", fn, reads=[s.reg(), self.cst.reg()] if q == 0 else (),
                              writes=[ps.reg()], inc=(q == 3))
                out = self.x.ap[:, g * KC + half * 4:g * KC + half * 4 + 4, j * 128:(j + 1) * 128]
                in_ = ps.ap[:, 0, :].rearrange("p (a b) -> p a b", a=4)
                eng = "dve" if half == 0 else "act"
                self.cp(eng, out, in_, [ps.reg()], [self.x.reg(g * KC + half * 4, g * KC + half * 4 + 4)])

    def store_tiles(self, src_fn, src_reg_fn):
        c = self.c
        st = [self.alloc(1, D, F32, at=self.scr0 + 16384 + i * 4096) for i in range(2)]
        for t in range(self.nt // 128):
            s = st[t % 2]
            g, j = divmod(t, 4)
            for half in range(2):
                ps = self.ps_next()
                for q in range(4):
                    kc = half * 4 + q
                    src = src_fn(g, kc)[:, j * 128:(j + 1) * 128]
                    fn = (lambda e, ps=ps, q=q, src=src:
                          e.transpose(ps.ap[:, 0, q * 128:(q + 1) * 128], src, self.identf))
                    self.c.op("pe", fn, reads=[src_reg_fn(g, kc), self.cst.reg()],
                              writes=[ps.reg()], inc=(q == 3))
                eng = "dve" if half == 0 else "act"
                self.cp(eng, s.ap[:, 0, half * 512:(half + 1) * 512], ps.ap[:, 0, :], [ps.reg()],
                        [(s.space, s.lo + half * 2048, s.lo + (half + 1) * 2048)])
            c.dma("sp", self.x_out[t * 128:(t + 1) * 128, :], s.ap[:, 0, :], reads=[s.reg()], writes=[("out", t, t + 1)])

    def store_x(self):
        self.store_tiles(lambda g, kc: self.x.ap[:, g * KC + kc, :], lambda g, kc: self.x.reg(g * KC + kc, g * KC + kc + 1))

    def final_norm_store(self):
        c = self.c
        base = self.scr0
        hf = self.alloc(KC, GT, F32, at=base)
        rb = self.alloc(1, GT, F32, at=base + 16384)
        sqt = [self.alloc(1, GT, BF16, at=base + 18432 + i * 1024) for i in range(2)]
        st = [self.alloc(1, D, F32, at=base + 20480 + i * 4096) for i in range(2)]
        for g in range(self.ng):
            self.rmsnorm(g * KC, GT, lambda kc: self.cst.ap[:, 0, CS_FN + kc:CS_FN + kc + 1], self.cst.reg(), hf, rb, sqt)
            for j in range(4):
                t = g * 4 + j
                s = st[t % 2]
                for half in range(2):
                    ps = self.ps_next()
                    for q in range(4):
                        kc = half * 4 + q
                        src = hf.ap[:, kc, j * 128:(j + 1) * 128]
                        fn = (lambda e, ps=ps, q=q, src=src:
                              e.transpose(ps.ap[:, 0, q * 128:(q + 1) * 128], src, self.identf))
                        self.c.op("pe", fn, reads=[hf.reg(), self.cst.reg()], writes=[ps.reg()], inc=(q == 3))
                    eng = "dve" if half == 0 else "act"
                    self.cp(eng, s.ap[:, 0, half * 512:(half + 1) * 512], ps.ap[:, 0, :], [ps.reg()],
                            [(s.space, s.lo + half * 2048, s.lo + (half + 1) * 2048)])
                c.dma("sp", self.x_out[t * 128:(t + 1) * 128, :], s.ap[:, 0, :], reads=[s.reg()], writes=[("out", t, t + 1)])

    def rmsnorm(self, xbase, n, gain_fn, gain_reg, hb, rb, sqt, xsrc=None, c0=0):
        xsrc = self.x if xsrc is None else xsrc
        ps = self.ps_next()
        for kc in range(KC):
            sq = sqt[kc % len(sqt)]
            xin = xsrc.ap[:, xbase + kc, c0:c0 + n]
            self.act(sq.ap[:, 0, 0:n], xin, AF.Square, [xsrc.reg(xbase + kc, xbase + kc + 1)], [sq.reg()])
            fn = (lambda e, ps=ps, sq=sq, kc=kc:
                  e.matmul(ps.ap[:, 0, 0:n], lhsT=self.ones_n.ap[:, 0, :], rhs=sq.ap[:, 0, 0:n], start=(kc == 0), stop=(kc == KC - 1)))
            if kc == KC - 1:
                self.c.op("pe", fn, reads=[sq.reg(), self.ones_n.reg()], writes=[ps.reg()])
            else:
                E = self.c.E["pe"]
                self.c._deps(E, [sq.reg(), self.ones_n.reg()], [ps.reg()] if kc == 0 else [])
                self.c.bump(E)
                self.c.pe_n = getattr(self.c, "pe_n", 0) + 1
                tok = Tok(E.sem, E.count, "pe")
                sem = E.sem
                E.prog.append(lambda e, fn=fn, sem=sem: fn(e).then_inc(sem, 1))
                self.c._commit(tok, [sq.reg(), self.ones_n.reg()], [])
        self.act(rb.ap[:, 0, 0:n], ps.ap[:, 0, 0:n], AF.Sqrt, [ps.reg()], [rb.reg()], bias=EPS, scale=1.0)
        self.c.op("dve", lambda e: e.reciprocal(out=rb.ap[:, 0, 0:n], in_=rb.ap[:, 0, 0:n]), [rb.reg()], [rb.reg()])
        for kc in range(KC):
            self.stt("dve", hb.ap[:, kc, 0:n], xsrc.ap[:, xbase + kc, c0:c0 + n], gain_fn(kc), rb.ap[:, 0, 0:n],
                     ALU.mult, ALU.mult,
                     [xsrc.reg(xbase + kc, xbase + kc + 1), gain_reg, rb.reg()], [hb.reg(kc, kc + 1)])

    def lin(self, W, wc0, nk, rhs_fn, reads, n=GT, ncol_blk=256, krow0=0, ps=None, out_ap=None, start=True, last=True):
        ps = self.ps_next() if ps is None else ps
        w3 = W.ap[:, 0, :]
        pairs = []
        for kc in range(nk):
            o = (krow0 + kc) * ncol_blk + wc0
            pairs.append((w3[:, o:o + 128], rhs_fn(kc)))
        out_ap = ps.ap[:, 0, 0:n] if out_ap is None else out_ap
        self.mm(ps, out_ap, pairs, [W.reg()] + list(reads), start=start, last=last)
        return ps

    def layer_small(self, l):
        c = self.c
        c.dma("sp", self.spp.ap[:, 0, :], self.spd[l * 128:(l + 1) * 128, :], writes=[self.spp.reg()])
        self.cp("pool", self.sgb_bf.ap[:, 0, :], self.spp.ap[:, 0, SP_SGB:SP_SGB + 512], [self.spp.reg()], [self.sgb_bf.reg()])

    def spc(self, off, n=1):
        return self.spp.ap[:, 0, off:off + n]

    def zero_state(self):
        for t in (self.TA, self.TB):
            self.c.op("pool", lambda e, t=t: e.memset(t.ap[:, 0, :], 0.0), (), [t.reg()])
        self.c.op("pool", lambda e: e.memset(self.z.ap[:, :, 0:2], 0.0), (), [self.z.reg()])
        for t in (self.QA, self.QB):
            self.c.op("pool", lambda e, t=t: e.memset(t.ap[:, :, :], 0.0), (), [t.reg()])

    def exchange(self, l):
        c = self.c
        n = self.ncores
        stt = self.stt_sb
        bin_, bout, sem = self.cc_in[l], self.cc_out[l], self.ccsem[l]
        c.dma("pool", bin_[:, :], stt.ap[:, 0, :], reads=[stt.reg()], writes=[("ccin", l, l + 1)])
        E = c.E["pool"]
        c._deps(E, [("ccin", l, l + 1)], [("ccout", l, l + 1)])
        E.prog.append(lambda e: e.collective_compute("AllGather", ALU.bypass, replica_groups=[list(range(n))],
                                                     ins=[bin_.ap().opt()], outs=[bout.ap().opt()]).then_inc(sem))
        c._commit(Tok(sem, 1, "cc"), [("ccin", l, l + 1)], [("ccout", l, l + 1)])
        gath = self.alloc(n, 260, F32, at=self.scr0)
        c.dma("sp", gath.ap[:, :, :], bout.ap().rearrange("(r p) f -> p r f", p=128), reads=[("ccout", l, l + 1)], writes=[gath.reg()])
        c.op("pool", lambda e: e.memset(stt.ap[:, 0, :], 0.0), (), [stt.reg()])
        for r in range(n):
            self.stt("dve", stt.ap[:, 0, :], gath.ap[:, r, :], self.cst.ap[:, 0, CS_SEL + r:CS_SEL + r + 1], stt.ap[:, 0, :],
                     ALU.mult, ALU.add, [gath.reg(r, r + 1), self.cst.reg(), stt.reg()], [stt.reg()])

    def state_from_stt(self):
        s = self.stt_sb
        self.cp("pool", self.TA.ap[:, 0, :], s.ap[:, 0, 0:128], [s.reg()], [self.TA.reg()])
        self.cp("pool", self.TB.ap[:, 0, :], s.ap[:, 0, 128:256], [s.reg()], [self.TB.reg()])
        self.cp("pool", self.z.ap[:, :, 0:2], s.ap[:, 0, 256:260].rearrange("p (a b) -> p a b", a=2), [s.reg()], [self.z.reg()])
        for t in (self.QA, self.QB):
            self.c.op("pool", lambda e, t=t: e.memset(t.ap[:, :, :], 0.0), (), [t.reg()])

    def alloc_mix(self):
        o = self.scr0
        m = {}

        def a(name, d1, d2, dt):
            nonlocal o
            t = self.alloc(d1, d2, dt, at=o)
            o += d1 * d2 * (4 if dt == F32 else 2)
            o = (o + 63) // 64 * 64
            m[name] = t
            return t
        a("hb", KC, MT, BF16)
        a("rb", 1, MT, F32)
        for i in range(2):
            a("sq%d" % i, 1, MT, BF16)
        a("sgate", 4, MT, BF16)
        a("ya", 2, MT, BF16)
        a("cin", 2, MT, BF16)
        ov = o
        a("tab", 4, MT, F32)
        for i in range(4):
            a("t%d" % i, 1, 512, F32)
        a("kA", 1, MT, BF16)
        a("kB", 1, MT, BF16)
        a("ktok", NCH, 256, BF16)
        a("vtok", NCH, 512, BF16)
        a("sT", 1, 512, BF16)
        a("obf", 1, 512, BF16)
        a("osq", 1, 512, BF16)
        a("u", 2, MT, BF16)
        a("vg", 1, 256, F32)
        a("vn", 1, 256, F32)
        a("vln", NCH, 256, BF16)
        a("st6", 1, 8, F32)
        self.mix_end = o
        o2 = ov
        for name, d1, d2, dt in (("mb", KC, MT, BF16), ("sg0", 1, MT, BF16), ("sg1", 1, MT, BF16),
                                 ("macc", 1, MT, F32), ("mtmp", 1, MT, F32)):
            m[name] = self.alloc(d1, d2, dt, at=o2)
            o2 += d1 * d2 * (4 if dt == F32 else 2)
        assert o2 <= self.mix_end, (o2, self.mix_end)
        return m

    def k_path(self, l, m, hb):
        c = self.c
        Wk = self.ws_get(l, "k")
        hk = lambda kc: hb.ap[:, kc, :]
        psA = self.lin(Wk, 0, KC, hk, [hb.reg()], n=MT)
        psB = self.lin(Wk, 128, KC, hk, [hb.reg()], n=MT)
        tab = m["tab"]
        kof = m.get("kof", 2)
        ck, sk = tab.ap[:, kof, :], tab.ap[:, kof + 1, :]
        t = [m["t%d" % i] for i in range(4)]
        a_, b_ = psA.ap[:, 0, 0:MT], psB.ap[:, 0, 0:MT]
        self.tt("dve", t[0].ap[:, 0, 0:MT], a_, ck, ALU.mult, [psA.reg(), tab.reg()], [t[0].reg()])
        self.tt("dve", t[1].ap[:, 0, 0:MT], b_, sk, ALU.mult, [psB.reg(), tab.reg()], [t[1].reg()])
        self.tt("dve", t[2].ap[:, 0, 0:MT], a_, sk, ALU.mult, [psA.reg(), tab.reg()], [t[2].reg()])
        self.tt("dve", t[3].ap[:, 0, 0:MT], b_, ck, ALU.mult, [psB.reg(), tab.reg()], [t[3].reg()])
        kA, kB = m["kA"], m["kB"]
        self.tt("pool", kA.ap[:, 0, :], t[0].ap[:, 0, 0:MT], t[1].ap[:, 0, 0:MT], ALU.subtract, [t[0].reg(), t[1].reg()], [kA.reg()])
        self.tt("pool", kB.ap[:, 0, :], t[2].ap[:, 0, 0:MT], t[3].ap[:, 0, 0:MT], ALU.add, [t[2].reg(), t[3].reg()], [kB.reg()])
        ps = self.ps_next()
        pb = self.ps_bf(ps)
        n = 0
        for j in range(NCH):
            for part, src in ((0, kA), (1, kB)):
                n += 1
                fn = (lambda e, pb=pb, j=j, part=part, src=src:
                      e.transpose(pb[:, j * 256 + part * 128:j * 256 + part * 128 + 128], src.ap[:, 0, j * 128:(j + 1) * 128], self.identb.ap[:, 0, :]))
                c.op("pe", fn, reads=[kA.reg(), kB.reg(), self.identb.reg()] if n == 1 else (), writes=[ps.reg()], inc=(n == 2 * NCH))
        ktok = m["ktok"]
        self.cp("act", ktok.ap[:, :, :], pb[:, 0:NCH * 256].rearrange("p (a b) -> p a b", a=NCH), [ps.reg()], [ktok.reg()])

    def v_path(self, l, m, hb):
        vtok = m["vtok"]
        for half, nm in enumerate(("v0", "v1")):
            W = self.ws_get(l, nm)
            w3 = W.ap[:, 0, :]
            for j in range(NCH):
                ps = self.ps_next()
                pairs = [(hb.ap[:, kc, j * 128:(j + 1) * 128], w3[:, kc * 256:(kc + 1) * 256]) for kc in range(KC)]
                self.mm(ps, ps.ap[:, 0, 0:256], pairs, [W.reg(), hb.reg()])
                eng = "act" if j % 2 == 0 else "dve"
                self.cp(eng, vtok.ap[:, j, half * 256:(half + 1) * 256], ps.ap[:, 0, 0:256], [ps.reg()],
                        [(vtok.space, vtok.lo + (j * 512 + half * 256) * 2, vtok.lo + (j * 512 + half * 256 + 256) * 2)])

    def state_update(self, m, j):
        ktok, vtok = m["ktok"], m["vtok"]
        G = self.cst.ap
        for part, T in ((0, self.TA), (1, self.TB)):
            ps = self.ps_next()
            self.mm(ps, ps.ap[:, 0, :], [(ktok.ap[:, j, part * 128:(part + 1) * 128], vtok.ap[:, j, :])],
                    [ktok.reg(j, j + 1), vtok.reg(j, j + 1)])
            for h in range(4):
                sl = slice(32 * h, 32 * h + 32)
                self.stt("dve", T.ap[sl, 0, :], T.ap[sl, 0, :], G[sl, 0, CS_G:CS_G + 1], ps.ap[sl, 0, h * 128:(h + 1) * 128],
                         ALU.mult, ALU.add, [T.reg(), ps.reg(), self.cst.reg()], [T.reg()])

    def load_tab(self, m, tok0):
        tab = m["tab"]
        f0 = 2 - m.get("kof", 2)
        src = self.tabs.ap().rearrange("(f p) t -> p f t", p=128)[:, f0:4, tok0:tok0 + MT]
        self.c.dma("sp", tab.ap[:, :, :], src, writes=[tab.reg()])

    def alloc_p1(self, idx):
        o = self.scr0 + idx * 16384
        m = {"kof": 0}

        def a(name, d1, d2, dt):
            nonlocal o
            m[name] = self.alloc(d1, d2, dt, at=o)
            o += d1 * d2 * (4 if dt == F32 else 2)
        a("hb", KC, MT, BF16)
        a("rb", 1, MT, F32)
        a("sq0", 1, MT, BF16)
        a("sq1", 1, MT, BF16)
        a("tab", 2, MT, F32)
        for i in range(4):
            a("t%d" % i, 1, MT, F32)
        a("kA", 1, MT, BF16)
        a("kB", 1, MT, BF16)
        a("ktok", NCH, 256, BF16)
        a("vtok", NCH, 512, BF16)
        assert o <= self.scr0 + (idx + 1) * 16384
        return m

    def p1(self, l):
        c = self.c
        sets = [self.alloc_p1(0), self.alloc_p1(1)]
        for g in range(self.ng):
            for sub in range(NSUB):
                m = sets[(g * NSUB + sub) % 2]
                hb, rb = m["hb"], m["rb"]
                sqt = [m["sq%d" % i] for i in range(2)]
                self.load_tab(m, g * GT + sub * MT)
                self.rmsnorm(g * KC, MT, lambda kc: self.spc(SP_GMIX + kc), self.spp.reg(), hb, rb, sqt, c0=sub * MT)
                self.k_path(l, m, hb)
                self.v_path(l, m, hb)
                for j in range(NCH):
                    self.state_update(m, j)
                if g == self.ng - 1 and sub == NSUB - 1:
                    Wx = self.ws_get(l, "ax")
                    Wc = self.ws_get(l, "ac")
                    s = self.stt_sb
                    t0 = m["t0"]
                    for ch in range(2):
                        psx = self.lin(Wx, ch * 128, KC, lambda kc: hb.ap[:, kc, MT - 2:MT], [hb.reg()], n=2)
                        psc = self.lin(Wc, ch * 128, KC, lambda kc: hb.ap[:, kc, MT - 2:MT], [hb.reg()], n=2)
                        self.cp("act", t0.ap[:, 0, 0:2], psx.ap[:, 0, 0:2], [psx.reg()], [t0.reg()])
                        self.tt("dve", s.ap[:, 0, 256 + 2 * ch:258 + 2 * ch], psc.ap[:, 0, 0:2], t0.ap[:, 0, 0:2], ALU.mult,
                                [psc.reg(), t0.reg()], [s.reg()])
        s = self.stt_sb
        self.cp("pool", s.ap[:, 0, 0:128], self.TA.ap[:, 0, :], [self.TA.reg()], [s.reg()])
        self.cp("pool", s.ap[:, 0, 128:256], self.TB.ap[:, 0, :], [self.TB.reg()], [s.reg()])

    def p1_seq(self, l):
        seq = []
        for g in range(self.ng):
            for sub in range(NSUB):
                seq += [(l, BLK[n]) for n in ["k", "v0", "v1"]]
        seq += [(l, BLK["ax"]), (l, BLK["ac"])]
        return seq

    def p2_seq(self, l):
        seq = [(l, BLK["xk%d" % j]) for j in range(4)] + [(l, BLK["xv%d" % j]) for j in range(4)] + [(l, BLK["wm"])]
        mix = ["qk", "k", "v0", "v1", "g0", "g1", "ax", "ac", "ab", "zu", "zv"]
        for o in range(8):
            mix += ["gab%d" % o, "gco%d" % o]
        mix += ["wo%d" % j for j in range(4)]
        per = []
        for j in range(4):
            per += ["xq%d" % j]
        per += ["xo%d" % j for j in range(4)]
        for half in range(2):
            for j in range(half * 6, min(11, half * 6 + 6)):
                per += ["w1_%d" % j, "w3_%d" % j]
            for o in range(8):
                per += ["w2%s%d" % ("ab"[half], o)]
        for g in range(self.ng):
            seq += [(l, BLK[n]) for n in mix] * NSUB
            seq += [(l, BLK[n]) for n in per]
        return seq

    def layer_prologue(self, l):
        c = self.c
        o = self.scr0
        mT = self.alloc(KC, MEM_LEN, F32, at=o)
        o += KC * MEM_LEN * 4
        mmT = self.alloc(KC, MEM_LEN, BF16, at=o)
        o += KC * MEM_LEN * 2
        rb = self.alloc(1, GT, F32, at=o)
        o += 2048
        sqt = [self.alloc(1, GT, BF16, at=o + i * 1024) for i in range(2)]
        o += 2048
        st = [self.alloc(1, D, F32, at=o + i * 4096) for i in range(2)]
        for t in range(MEM_LEN // 128):
            s = st[t % 2]
            c.dma("sp", s.ap[:, 0, :], self.mem_d[t * 128:(t + 1) * 128, :], writes=[s.reg()])
            for half in range(2):
                ps = self.ps_next()
                for q in range(4):
                    kc = half * 4 + q
                    fn = (lambda e, ps=ps, q=q, s=s, kc=kc:
                          e.transpose(ps.ap[:, 0, q * 128:(q + 1) * 128], s.ap[:, 0, kc * 128:(kc + 1) * 128], self.identf))
                    c.op("pe", fn, reads=[s.reg(), self.cst.reg()], writes=[ps.reg()], inc=(q == 3))
                out = mT.ap[:, half * 4:half * 4 + 4, t * 128:(t + 1) * 128]
                self.cp("dve", out, ps.ap[:, 0, :].rearrange("p (a b) -> p a b", a=4), [ps.reg()], [mT.reg(half * 4, half * 4 + 4)])
        self.rmsnorm(0, MEM_LEN, lambda kc: self.spc(SP_GMEM + kc), self.spp.reg(), mmT, rb, sqt, xsrc=mT)
        for j in range(4):
            W = self.ws_get(l, "xk%d" % j)
            for ch in range(2):
                ps = self.lin(W, ch * 128, KC, lambda kc: mmT.ap[:, kc, :], [mmT.reg()], n=MEM_LEN)
                self.cp("act", self.kmemT.ap[:, j * 2 + ch, :], ps.ap[:, 0, 0:MEM_LEN], [ps.reg()], [self.kmemT.reg(j * 2 + ch, j * 2 + ch + 1)])
        for j in range(4):
            W = self.ws_get(l, "xv%d" % j)
            w3 = W.ap[:, 0, :]
            for mt in range(2):
                ps = self.ps_next()
                pairs = [(mmT.ap[:, kc, mt * 128:(mt + 1) * 128], w3[:, kc * 256:(kc + 1) * 256]) for kc in range(KC)]
                self.mm(ps, ps.ap[:, 0, 0:256], pairs, [W.reg(), mmT.reg()])
                self.cp("act", self.vmem.ap[:, mt, j * 256:(j + 1) * 256], ps.ap[:, 0, 0:256], [ps.reg()],
                        [(self.vmem.space, self.vmem.lo + (mt * D + j * 256) * 2, self.vmem.lo + (mt * D + j * 256 + 256) * 2)])
        W = self.ws_get(l, "wm")
        self.tt("pool", self.wmT.ap[:, 0, :], W.ap[:, 0, 0:512], self.maskb.ap[:, 0, :], ALU.mult,
                [W.reg(), self.maskb.reg()], [self.wmT.reg()])

    def mark(self, label):
        if not hasattr(self, "marks"):
            self.marks = []
        self.marks.append((label, getattr(self.c, "pe_n", 0)))

    def p2(self, l):
        self.mark("prologue L%d" % l)
        self.layer_prologue(l)
        m = self.alloc_mix()
        for g in range(self.ng):
            if "nomix" not in DBG:
                for sub in range(NSUB):
                    self.mark("mix L%d g%d s%d" % (l, g, sub))
                    self.mix_group(l, g, sub, m)
            if "noxa" not in DBG:
                self.mark("xattn L%d g%d" % (l, g))
                self.xattn_group(l, g)
            if "noffn" not in DBG:
                self.mark("ffn L%d g%d" % (l, g))
                self.ffn_group(l, g)
        self.mark("end L%d" % l)

    def mix_group(self, l, g, sub, m):
        c = self.c
        hb, rb = m["hb"], m["rb"]
        sqt = [m["sq%d" % i] for i in range(2)]
        t = [m["t%d" % i] for i in range(4)]
        hk = lambda kc: hb.ap[:, kc, :]
        c0 = sub * MT
        self.load_tab(m, g * GT + c0)
        self.rmsnorm(g * KC, MT, lambda kc: self.spc(SP_GMIX + kc), self.spp.reg(), hb, rb, sqt, c0=c0)
        tab = m["tab"]
        Wq = self.ws_get(l, "qk")
        psA = self.lin(Wq, 0, KC, hk, [hb.reg()], n=MT)
        psB = self.lin(Wq, 128, KC, hk, [hb.reg()], n=MT)
        cq, sq_ = tab.ap[:, 0, :], tab.ap[:, 1, :]
        a_, b_ = psA.ap[:, 0, 0:MT], psB.ap[:, 0, 0:MT]
        self.tt("dve", t[0].ap[:, 0, 0:MT], a_, cq, ALU.mult, [psA.reg(), tab.reg()], [t[0].reg()])
        self.tt("dve", t[1].ap[:, 0, 0:MT], b_, sq_, ALU.mult, [psB.reg(), tab.reg()], [t[1].reg()])
        self.tt("dve", t[2].ap[:, 0, 0:MT], a_, sq_, ALU.mult, [psA.reg(), tab.reg()], [t[2].reg()])
        self.tt("dve", t[3].ap[:, 0, 0:MT], b_, cq, ALU.mult, [psB.reg(), tab.reg()], [t[3].reg()])
        for Q, a, b, op in ((self.QA, t[0], t[1], ALU.subtract), (self.QB, t[2], t[3], ALU.add)):
            q4 = Q.ap.rearrange("p j (h c) -> p j h c", h=4)
            for h in range(4):
                sl = slice(32 * h, 32 * h + 32)
                out = q4[sl, :, h, :]
                i0 = a.ap[sl, 0, 0:MT].rearrange("p (j c) -> p j c", j=NCH)
                i1 = b.ap[sl, 0, 0:MT].rearrange("p (j c) -> p j c", j=NCH)
                self.tt("pool", out, i0, i1, op, [a.reg(), b.reg()], [Q.reg()])
        self.k_path(l, m, hb)
        self.v_path(l, m, hb)
        sgate = m["sgate"]
        for half, nm in enumerate(("g0", "g1")):
            W = self.ws_get(l, nm)
            for ch in range(2):
                h = half * 2 + ch
                ps = self.lin(W, ch * 128, KC, hk, [hb.reg()], n=MT)
                self.act(sgate.ap[:, h, :], ps.ap[:, 0, 0:MT], AF.Silu, [ps.reg()], [sgate.reg(h, h + 1)])
                self.ts("dve", sgate.ap[:, h, :], sgate.ap[:, h, :], self.spc(SP_GN + h), None, ALU.mult, None,
                        [sgate.reg(h, h + 1), self.spp.reg()], [sgate.reg(h, h + 1)])
        z, ya = self.z, m["ya"]
        Wx = self.ws_get(l, "ax")
        psx = [self.lin(Wx, ch * 128, KC, hk, [hb.reg()], n=MT) for ch in range(2)]
        Wc = self.ws_get(l, "ac")
        for ch in range(2):
            self.cp("act", t[ch].ap[:, 0, 0:MT], psx[ch].ap[:, 0, 0:MT], [psx[ch].reg()], [t[ch].reg()])
            psc = self.lin(Wc, ch * 128, KC, hk, [hb.reg()], n=MT)
            self.tt("dve", z.ap[:, ch, 2:MT + 2], psc.ap[:, 0, 0:MT], t[ch].ap[:, 0, 0:MT], ALU.mult, [psc.reg(), t[ch].reg()], [z.reg(ch, ch + 1)])
        Wb = self.ws_get(l, "ab")
        for ch in range(2):
            acc = t[2 + ch]
            av = acc.ap[:, 0, 0:MT]
            cw = lambda k, ch=ch: self.spc(SP_CONV + ch * 3 + k)
            self.ts("dve", av, z.ap[:, ch, 0:MT], cw(0), None, ALU.mult, None, [z.reg(ch, ch + 1), self.spp.reg()], [acc.reg()])
            self.stt("dve", av, z.ap[:, ch, 1:MT + 1], cw(1), av, ALU.mult, ALU.add,
                     [z.reg(ch, ch + 1), self.spp.reg(), acc.reg()], [acc.reg()])
            self.stt("dve", av, z.ap[:, ch, 2:MT + 2], cw(2), av, ALU.mult, ALU.add,
                     [z.reg(ch, ch + 1), self.spp.reg(), acc.reg()], [acc.reg()])
            psb = self.lin(Wb, ch * 128, KC, hk, [hb.reg()], n=MT)
            self.tt("dve", ya.ap[:, ch, :], psb.ap[:, 0, 0:MT], av, ALU.mult, [psb.reg(), acc.reg()], [ya.reg(ch, ch + 1)])
            self.cp("pool", z.ap[:, ch, 0:2], z.ap[:, ch, MT:MT + 2], [z.reg(ch, ch + 1)], [z.reg(ch, ch + 1)])
        u, cin, vg, vn, vln, st6 = m["u"], m["cin"], m["vg"], m["vn"], m["vln"], m["st6"]
        W = self.ws_get(l, "zu")
        for ch in range(2):
            ps = self.lin(W, ch * 128, KC, hk, [hb.reg()], n=MT)
            self.act(u.ap[:, ch, :], ps.ap[:, 0, 0:MT], AF.Gelu, [ps.reg()], [u.reg(ch, ch + 1)])
        W = self.ws_get(l, "zv")
        w3 = W.ap[:, 0, :]
        for j in range(NCH):
            ps = self.ps_next()
            pairs = [(hb.ap[:, kc, j * 128:(j + 1) * 128], w3[:, kc * 256:(kc + 1) * 256]) for kc in range(KC)]
            self.mm(ps, ps.ap[:, 0, 0:256], pairs, [W.reg(), hb.reg()])
            self.act(vg.ap[:, 0, :], ps.ap[:, 0, 0:256], AF.Gelu, [ps.reg()], [vg.reg()])
            c.op("dve", lambda e: e.bn_stats(out=st6.ap[:, 0, 0:6], in_=vg.ap[:, 0, :]), [vg.reg()], [st6.reg()])
            c.op("dve", lambda e: e.bn_aggr(out=st6.ap[:, 0, 6:8], in_=st6.ap[:, 0, 0:6]), [st6.reg()], [st6.reg()])
            self.act(st6.ap[:, 0, 7:8], st6.ap[:, 0, 7:8], AF.Sqrt, [st6.reg()], [st6.reg()], bias=EPS, scale=1.0)
            c.op("dve", lambda e: e.reciprocal(out=st6.ap[:, 0, 7:8], in_=st6.ap[:, 0, 7:8]), [st6.reg()], [st6.reg()])
            self.ts("dve", vn.ap[:, 0, :], vg.ap[:, 0, :], st6.ap[:, 0, 6:7], st6.ap[:, 0, 7:8], ALU.subtract, ALU.mult,
                    [vg.reg(), st6.reg()], [vn.reg()])
            self.tt("pool", vln.ap[:, j, :], vn.ap[:, 0, :], self.spc(SP_LN, 256), ALU.mult, [vn.reg(), self.spp.reg()], [vln.reg(j, j + 1)])
        for j in range(NCH):
            for pr in range(2):
                ps = self.ps_next()
                self.mm(ps, ps.ap[:, 0, 0:256],
                        [(self.ones_1.ap[0:1, 0, :], self.sgb_bf.ap[0:1, 0, pr * 256:(pr + 1) * 256]),
                         (vln.ap[:, j, pr * 128:(pr + 1) * 128], self.wmT.ap[:, 0, pr * 256:(pr + 1) * 256])],
                        [self.ones_1.reg(), self.sgb_bf.reg(), vln.reg(j, j + 1), self.wmT.reg()])
                for gi in range(2):
                    sl = slice(64 * gi, 64 * gi + 64)
                    self.tt("dve", cin.ap[sl, pr, j * 128:(j + 1) * 128], ps.ap[sl, 0, gi * 128:(gi + 1) * 128],
                            u.ap[sl, pr, j * 128:(j + 1) * 128], ALU.mult, [ps.reg(), u.reg(pr, pr + 1)], [cin.reg(pr, pr + 1)])
        sT, obf, osq = m["sT"], m["obf"], m["osq"]
        kA, kB, vtok = m["kA"], m["kB"], m["vtok"]
        G = self.cst.ap
        for j in range(NCH):
            for R, T in ((self.RA, self.TA), (self.RB, self.TB)):
                self.ts("dve", R.ap[:, 0, :], T.ap[:, 0, :], G[:, 0, CS_G:CS_G + 1], None, ALU.mult, None,
                        [T.reg(), self.cst.reg()], [R.reg()])
            self.state_update(m, j)
            pss = self.ps_next()
            self.mm(pss, pss.ap[:, 0, :],
                    [(kA.ap[:, 0, j * 128:(j + 1) * 128], self.QA.ap[:, j, :]),
                     (kB.ap[:, 0, j * 128:(j + 1) * 128], self.QB.ap[:, j, :])],
                    [kA.reg(), kB.reg(), self.QA.reg(j, j + 1), self.QB.reg(j, j + 1)])
            self.tt("dve", sT.ap[:, 0, :], pss.ap[:, 0, :], self.cst.ap[:, 0, CS_MASK:CS_MASK + 512], ALU.mult,
                    [pss.reg(), self.cst.reg()], [sT.reg()])
            pso = self.ps_next()
            self.mm(pso, pso.ap[:, 0, :],
                    [(self.RA.ap[:, 0, :], self.QA.ap[:, j, :]), (self.RB.ap[:, 0, :], self.QB.ap[:, j, :])],
                    [self.RA.reg(), self.RB.reg(), self.QA.reg(j, j + 1), self.QB.reg(j, j + 1)], last=False)
            for h in range(4):
                self.mm(pso, pso.ap[:, 0, h * 128:(h + 1) * 128],
                        [(vtok.ap[:, j, h * 128:(h + 1) * 128], sT.ap[:, 0, h * 128:(h + 1) * 128])],
                        [vtok.reg(j, j + 1), sT.reg()], start=False, last=(h == 3), first_writes=False)
            self.cp("act", obf.ap[:, 0, :], pso.ap[:, 0, :], [pso.reg()], [obf.reg()])
            self.act(osq.ap[:, 0, :], pso.ap[:, 0, :], AF.Square, [pso.reg()], [osq.reg()])
            psm = self.ps_next()
            self.mm(psm, psm.ap[:, 0, :], [(self.ones_g.ap[:, 0, :], obf.ap[:, 0, :])], [self.ones_g.reg(), obf.reg()])
            psq = self.ps_next()
            self.mm(psq, psq.ap[:, 0, :], [(self.ones_g.ap[:, 0, :], osq.ap[:, 0, :])], [self.ones_g.reg(), osq.reg()])
            A_, B_ = t[0], t[1]
            self.act(A_.ap[:, 0, :], psm.ap[:, 0, :], AF.Square, [psm.reg()], [A_.reg()])
            self.tt("dve", A_.ap[:, 0, :], psq.ap[:, 0, :], A_.ap[:, 0, :], ALU.subtract, [psq.reg(), A_.reg()], [A_.reg()])
            self.ts("dve", A_.ap[:, 0, :], A_.ap[:, 0, :], 0.0, None, ALU.max, None, [A_.reg()], [A_.reg()])
            self.act(A_.ap[:, 0, :], A_.ap[:, 0, :], AF.Sqrt, [A_.reg()], [A_.reg()], bias=EPS, scale=1.0)
            c.op("dve", lambda e, A_=A_: e.reciprocal(out=A_.ap[:, 0, :], in_=A_.ap[:, 0, :]), [A_.reg()], [A_.reg()])
            self.tt("dve", B_.ap[:, 0, :], obf.ap[:, 0, :], psm.ap[:, 0, :], ALU.subtract, [obf.reg(), psm.reg()], [B_.reg()])
            self.tt("pool", B_.ap[:, 0, :], B_.ap[:, 0, :], A_.ap[:, 0, :], ALU.mult, [B_.reg(), A_.reg()], [B_.reg()])
            rg3 = sgate.ap[:, :, j * 128:(j + 1) * 128]
            self.tt("pool", rg3, B_.ap[:, 0, :].rearrange("p (h c) -> p h c", h=4), rg3, ALU.mult, [B_.reg(), sgate.reg()], [sgate.reg()])
        mb, sg0, sg1, macc, mtmp = m["mb"], m["sg0"], m["sg1"], m["macc"], m["mtmp"]
        for o in range(8):
            Wab = self.ws_get(l, "gab%d" % o)
            Wco = self.ws_get(l, "gco%d" % o)
            ps = self.lin(Wab, 0, KC, hk, [hb.reg()], n=MT)
            self.act(sg0.ap[:, 0, :], ps.ap[:, 0, 0:MT], AF.Sigmoid, [ps.reg()], [sg0.reg()])
            ps = self.lin(Wco, 0, 2, lambda r: ya.ap[:, r, :], [ya.reg()], ncol_blk=128, krow0=8, n=MT)
            self.tt("dve", macc.ap[:, 0, :], ps.ap[:, 0, 0:MT], sg0.ap[:, 0, :], ALU.mult, [ps.reg(), sg0.reg()], [macc.reg()])
            if "noa" in DBG:
                self.c.op("pool", lambda e: e.memset(macc.ap[:, 0, :], 0.0), (), [macc.reg()])
            ps = self.lin(Wab, 128, KC, hk, [hb.reg()], n=MT)
            self.act(sg1.ap[:, 0, :], ps.ap[:, 0, 0:MT], AF.Sigmoid, [ps.reg()], [sg1.reg()])
            ps = self.lin(Wco, 0, 4, lambda r: sgate.ap[:, r, :], [sgate.reg()], ncol_blk=128, krow0=12, n=MT)
            self.tt("dve", mtmp.ap[:, 0, :], ps.ap[:, 0, 0:MT], sg1.ap[:, 0, :], ALU.mult, [ps.reg(), sg1.reg()], [mtmp.reg()])
            if "nob" not in DBG:
                self.tt("pool", macc.ap[:, 0, :], macc.ap[:, 0, :], mtmp.ap[:, 0, :], ALU.add, [macc.reg(), mtmp.reg()], [macc.reg()])
            ps = self.lin(Wco, 0, KC, hk, [hb.reg()], ncol_blk=128, n=MT)
            self.act(sg0.ap[:, 0, :], ps.ap[:, 0, 0:MT], AF.Sigmoid, [ps.reg()], [sg0.reg()])
            ps = self.lin(Wco, 0, 2, lambda r: cin.ap[:, r, :], [cin.reg()], ncol_blk=128, krow0=10, n=MT)
            self.tt("dve", mtmp.ap[:, 0, :], ps.ap[:, 0, 0:MT], sg0.ap[:, 0, :], ALU.mult, [ps.reg(), sg0.reg()], [mtmp.reg()])
            if "noc" in DBG:
                self.c.op("pool", lambda e: e.memset(mtmp.ap[:, 0, :], 0.0), (), [mtmp.reg()])
            self.tt("pool", mb.ap[:, o, :], macc.ap[:, 0, :], mtmp.ap[:, 0, :], ALU.add, [macc.reg(), mtmp.reg()], [mb.reg(o, o + 1)])
        for jb in range(4):
            W = self.ws_get(l, "wo%d" % jb)
            for ch in range(2):
                oc = jb * 2 + ch
                ps = self.lin(W, ch * 128, KC, lambda kc: mb.ap[:, kc, :], [mb.reg()], n=MT)
                xr = self.x.reg(g * KC + oc, g * KC + oc + 1)
                xv = self.x.ap[:, g * KC + oc, c0:c0 + MT]
                self.tt("dve", xv, ps.ap[:, 0, 0:MT], xv, ALU.add, [ps.reg(), xr], [xr])

    def xattn_group(self, l, g):
        c = self.c
        o = self.scr0

        def a(d1, d2, dt):
            nonlocal o
            t = self.alloc(d1, d2, dt, at=o)
            o += d1 * d2 * (4 if dt == F32 else 2)
            o = (o + 63) // 64 * 64
            return t
        hb = a(KC, GT, BF16)
        rb = a(1, GT, F32)
        sqt = [a(1, GT, BF16) for _ in range(2)]
        qT = [a(2, GT, BF16) for _ in range(2)]
        eT = [a(2, GT, BF16) for _ in range(2)]
        rden = a(1, GT, F32)
        oT = a(KC, GT, BF16)
        hk = lambda kc: hb.ap[:, kc, :]
        self.rmsnorm(g * KC, GT, lambda kc: self.spc(SP_GXA + kc), self.spp.reg(), hb, rb, sqt)
        scale = 256.0 ** -0.5
        for hd in range(4):
            W = self.ws_get(l, "xq%d" % hd)
            q = qT[hd % 2]
            for ch in range(2):
                ps = self.lin(W, ch * 128, KC, hk, [hb.reg()])
                self.cp("act", q.ap[:, ch, :], ps.ap[:, 0, :], [ps.reg()], [q.reg(ch, ch + 1)])
            e = eT[hd % 2]
            rd = rden
            for mh in range(2):
                ps = self.ps_next()
                pairs = [(self.kmemT.ap[:, 2 * hd + dc, mh * 128:(mh + 1) * 128], q.ap[:, dc, :]) for dc in range(2)]
                self.mm(ps, ps.ap[:, 0, :], pairs, [self.kmemT.reg(2 * hd, 2 * hd + 2), q.reg()])
                self.act(e.ap[:, mh, :], ps.ap[:, 0, :], AF.Exp, [ps.reg()], [e.reg(mh, mh + 1)], scale=scale)
            psd = self.ps_next()
            self.mm(psd, psd.ap[:, 0, :], [(self.ones_1.ap[:, 0, :], e.ap[:, 0, :]), (self.ones_1.ap[:, 0, :], e.ap[:, 1, :])],
                    [self.ones_1.reg(), e.reg()])
            c.op("dve", lambda e_, rd=rd, psd=psd: e_.reciprocal(out=rd.ap[:, 0, :], in_=psd.ap[:, 0, :]), [psd.reg()], [rd.reg()])
            for dc in range(2):
                ps = self.ps_next()
                pairs = [(self.vmem.ap[:, mh, (2 * hd + dc) * 128:(2 * hd + dc + 1) * 128], e.ap[:, mh, :]) for mh in range(2)]
                self.mm(ps, ps.ap[:, 0, :], pairs, [self.vmem.reg(), e.reg()])
                self.tt("dve", oT.ap[:, 2 * hd + dc, :], ps.ap[:, 0, :], rd.ap[:, 0, :], ALU.mult, [ps.reg(), rd.reg()],
                        [oT.reg(2 * hd + dc, 2 * hd + dc + 1)])
        for jb in range(4):
            W = self.ws_get(l, "xo%d" % jb)
            for ch in range(2):
                oc = jb * 2 + ch
                ps = self.lin(W, ch * 128, KC, lambda kc: oT.ap[:, kc, :], [oT.reg()])
                xr = self.x.reg(g * KC + oc, g * KC + oc + 1)
                self.tt("dve", self.x.ap[:, g * KC + oc, :], ps.ap[:, 0, :], self.x.ap[:, g * KC + oc, :], ALU.add, [ps.reg(), xr], [xr])

    def ffn_group(self, l, g):
        o = self.scr0

        def a(d1, d2, dt):
            nonlocal o
            t = self.alloc(d1, d2, dt, at=o)
            o += d1 * d2 * (4 if dt == F32 else 2)
            o = (o + 63) // 64 * 64
            return t
        hb = a(KC, GT, BF16)
        rb = a(1, GT, F32)
        sqt = [a(1, GT, BF16) for _ in range(2)]
        hm = a(12, GT, BF16)
        st_ = [a(1, GT, BF16) for _ in range(2)]
        hk = lambda kc: hb.ap[:, kc, :]
        self.rmsnorm(g * KC, GT, lambda kc: self.spc(SP_GFFN + kc), self.spp.reg(), hb, rb, sqt)
        for half in range(2):
            j0, j1 = half * 6, min(11, half * 6 + 6)
            base = 12 * half
            for j in range(j0, j1):
                W1 = self.ws_get(l, "w1_%d" % j)
                W3 = self.ws_get(l, "w3_%d" % j)
                for ch in range(2):
                    fc = 2 * j + ch - base
                    s = st_[fc % 2]
                    ps1 = self.lin(W1, ch * 128, KC, hk, [hb.reg()])
                    self.act(s.ap[:, 0, :], ps1.ap[:, 0, :], AF.Silu, [ps1.reg()], [s.reg()])
                    ps3 = self.lin(W3, ch * 128, KC, hk, [hb.reg()])
                    self.tt("dve", hm.ap[:, fc, :], ps3.ap[:, 0, :], s.ap[:, 0, :], ALU.mult, [ps3.reg(), s.reg()], [hm.reg(fc, fc + 1)])
            nk = 12 if half == 0 else 10
            for oc in range(8):
                W = self.ws_get(l, "w2%s%d" % ("ab"[half], oc))
                ps = self.lin(W, 0, nk, lambda kc: hm.ap[:, kc, :], [hm.reg(0, nk)], ncol_blk=128)
                xr = self.x.reg(g * KC + oc, g * KC + oc + 1)
                self.tt("dve", self.x.ap[:, g * KC + oc, :], ps.ap[:, 0, :], self.x.ap[:, g * KC + oc, :], ALU.add, [ps.reg(), xr], [xr])


_PROG = {}


def get_prog(nt, mode, nlayers=1, ncores=NCORES):
    key = (nt, mode, nlayers, ncores)
    if key not in _PROG:
        seq = Builder(nt, mode, nlayers=nlayers, ncores=ncores).ws_rec
        _PROG[key] = Builder(nt, mode, nlayers=nlayers, seq=seq, ncores=ncores).nc
    return _PROG[key]


def run_fused(x, mem, layers, final_norm, ncores=NCORES):
    B, S, _ = x.shape
    per_b = ncores // B
    nt = S // per_b
    cores = list(range(ncores))
    nl = len(layers)
    pk = np.concatenate([L["pack"].reshape(NBLK * 128, SLOT_E) for L in layers], axis=0)
    sp = np.concatenate([L["small"] for L in layers], axis=0)
    nc = get_prog(nt, "fused", nlayers=nl, ncores=ncores)
    in_maps = []
    for c in cores:
        in_maps.append({
            "x_in": np.ascontiguousarray(x[c // per_b, (c % per_b) * nt:(c % per_b + 1) * nt]),
            "cst": make_consts_fused(c, per_b, final_norm),
            "wpk": pk, "spd": sp,
            "tabs": make_tables((c % per_b) * nt, nt).reshape(4 * 128, nt),
            "mem": np.ascontiguousarray(mem[c // per_b]),
        })
    res = run_bass_kernel_spmd(nc, in_maps, core_ids=cores)
    out = np.zeros_like(x)
    for c in cores:
        out[c // per_b, (c % per_b) * nt:(c % per_b + 1) * nt] = res.results[c]["x_out"]
    return out


def make_consts_fused(core, per_b, final_norm):
    cs = make_consts(0, final_norm)
    cs[:, CS_SEL:CS_SEL + 8] = 0.0
    if core % per_b != 0:
        cs[:, CS_SEL + core - 1] = 1.0
    return cs


def run_model(x, mem, layers, final_norm, ncores=NCORES):
    B, S, _ = x.shape
    per_b = ncores // B
    nt = S // per_b
    cores = list(range(ncores))
    xs = [np.ascontiguousarray(x[c // per_b, (c % per_b) * nt:(c % per_b + 1) * nt]) for c in cores]
    mems = [np.ascontiguousarray(mem[c // per_b]) for c in cores]
    csts = [make_consts(c % per_b, final_norm) for c in cores]
    tabs = [make_tables((c % per_b) * nt, nt).reshape(4 * 128, nt) for c in cores]
    for L in layers:
        pk = L["pack"].reshape(NBLK * 128, SLOT_E)
        sp = L["small"]
        if per_b > 1:
            nc1 = get_prog(nt, "p1")
            pk1 = np.ascontiguousarray(L["pack"][[BLK[n] for n in P1_BLOCKS]]).reshape(len(P1_BLOCKS) * 128, SLOT_E)
            r1 = run_bass_kernel_spmd(nc1, [{"x_in": xs[c], "cst": csts[c], "wpk": pk1, "spd": sp, "tabs": tabs[c]} for c in cores],
                                      core_ids=cores)
            st = [r["st_out"] for r in r1.results]
        zero = np.zeros((128, 260), np.float32)
        st_in = []
        for c in cores:
            if c % per_b == 0:
                st_in.append(zero)
            else:
                st_in.append(np.ascontiguousarray(st[c - 1]))
        nc2 = get_prog(nt, "p2")
        r2 = run_bass_kernel_spmd(nc2, [{"x_in": xs[c], "cst": csts[c], "wpk": pk, "spd": sp, "tabs": tabs[c],
                                         "mem": mems[c], "st_in": st_in[c]} for c in cores], core_ids=cores)
        xs = [r["x_out"] for r in r2.results]
    nc3 = get_prog(nt, "fin")
    r3 = run_bass_kernel_spmd(nc3, [{"x_in": xs[c], "cst": csts[c]} for c in cores], core_ids=cores)
    out = np.zeros_like(x)
    for c in cores:
        out[c // per_b, (c % per_b) * nt:(c % per_b + 1) * nt] = r3.results[c]["x_out"]
    return out


def prep_layers(norm_mix, w_in, conv_w, w_a_out, ret_gn, w_b_out, sg_ln, sg_w, sg_b, w_c_out, w_o,
                norm_xa, norm_mem, xa_q, xa_k, xa_v, xa_o, norm_ffn, w1, w3, w2, nl):
    layers = []
    for l in range(nl):
        layers.append(dict(
            pack=pack_layer(w_in[l], w_a_out[l], w_b_out[l], w_c_out[l], w_o[l], xa_q[l], xa_k[l], xa_v[l], xa_o[l],
                            w1[l], w3[l], w2[l], sg_w[l]),
            small=pack_small(norm_mix[l], norm_xa[l], norm_ffn[l], norm_mem[l], conv_w[l], ret_gn[l], sg_ln[l], sg_b[l])))
    return layers


def kernel(x, mem, norm_mix, w_in, conv_w, w_a_out, ret_gn, w_b_out, sg_ln, sg_w, sg_b,
           w_c_out, w_o, norm_xa, norm_mem, xa_q, xa_k, xa_v, xa_o, norm_ffn, w1, w3, w2, final_norm):
    f = lambda a: np.asarray(a, dtype=np.float32)
    args = [f(a) for a in (norm_mix, w_in, conv_w, w_a_out, ret_gn, w_b_out, sg_ln, sg_w, sg_b, w_c_out, w_o,
                           norm_xa, norm_mem, xa_q, xa_k, xa_v, xa_o, norm_ffn, w1, w3, w2)]
    layers = prep_layers(*args, nl=DEPTH)
    return run_fused(f(x), f(mem), layers, f(final_norm))
```
